# Optimizing a Trainium2 kernel written in Bass

```python
import math
import jax
import jax.numpy as jnp
from jax import lax
import numpy as np

D_MODEL = 1024
BATCH = 2
SEQ = 8192
DEPTH = 2

HEAD_DIM = 64
BRANCH_WIDTH = 512
N_BRANCH = 4
SSM_HEADS = 8
SSM_HEAD_DIM = 64
SSM_INNER = SSM_HEADS * SSM_HEAD_DIM
SSM_GROUPS = 2
SSM_STATE = 128
SSM_CONV = 4
SSM_CHUNK = 256
SSM_CONV_CH = SSM_INNER + 2 * SSM_GROUPS * SSM_STATE
FOX_HEADS = 8
MOBA_HEADS = 8
MOBA_BLOCK = 256
MOBA_TOPK = 3
MOBA_Q_BLOCK = 64
DSA_HEADS = 8
IDX_HEADS = 4
IDX_DIM = 64
DSA_TOPK = 256
Q_BLOCK = 128
ROPE_THETA = 10000.0
FFN_HIDDEN = -(-8 * D_MODEL // (3 * 256)) * 256
EPS = 1e-6

IN_SPLITS = (
    SSM_INNER, SSM_INNER, SSM_GROUPS * SSM_STATE, SSM_GROUPS * SSM_STATE, SSM_HEADS,
    FOX_HEADS * HEAD_DIM, FOX_HEADS * HEAD_DIM, FOX_HEADS * HEAD_DIM, FOX_HEADS,
    MOBA_HEADS * HEAD_DIM, MOBA_HEADS * HEAD_DIM, MOBA_HEADS * HEAD_DIM,
    DSA_HEADS * HEAD_DIM, DSA_HEADS * HEAD_DIM, DSA_HEADS * HEAD_DIM,
    IDX_HEADS * IDX_DIM, IDX_DIM, IDX_HEADS,
)
D_IN = sum(IN_SPLITS)

kernel_name = 'hybrid_gated_ssd_fox_moba_dsa'


def rms_norm(x, g):
    x32 = x.astype(jnp.float32)
    y = x32 * lax.rsqrt(jnp.mean(x32 * x32, axis=-1, keepdims=True) + EPS)
    return (y * g.astype(jnp.float32)).astype(x.dtype)


def rope_tables(seq):
    pos = jnp.arange(seq, dtype=jnp.float32)
    inv = ROPE_THETA ** (-jnp.arange(0, HEAD_DIM, 2, dtype=jnp.float32) / HEAD_DIM)
    ang = pos[:, None] * inv[None, :]
    return jnp.cos(ang), jnp.sin(ang)


def apply_rope(t, cos, sin):
    t1, t2 = jnp.split(t.astype(jnp.float32), 2, axis=-1)
    c = cos[None, :, None, :]
    s = sin[None, :, None, :]
    return jnp.concatenate([t1 * c - t2 * s, t2 * c + t1 * s], axis=-1).astype(t.dtype)


def split_heads(t, n_heads):
    return t.reshape(t.shape[0], t.shape[1], n_heads, -1)


def causal_dwconv(u, w, b):
    width = w.shape[0]
    up = jnp.pad(u, ((0, 0), (width - 1, 0), (0, 0)))
    y = lax.conv_general_dilated(up, w[:, None, :], window_strides=(1,), padding='VALID',
                                 dimension_numbers=('NWC', 'WIO', 'NWC'),
                                 feature_group_count=u.shape[-1])
    return y + b


def ssd_chunked(x, dt, a, bmat, cmat):
    bsz, seq, n_heads, hp = x.shape
    pad = (-seq) % SSM_CHUNK
    padl = lambda t: jnp.pad(t, ((0, 0), (0, pad)) + ((0, 0),) * (t.ndim - 2))
    n_chunk = (seq + pad) // SSM_CHUNK
    chunk = lambda t: padl(t).reshape((bsz, n_chunk, SSM_CHUNK) + t.shape[2:])
    xc, dtc, bc, cc = chunk(x), chunk(dt), chunk(bmat), chunk(cmat)
    a_cs = jnp.cumsum((dtc * a).transpose(0, 3, 1, 2), axis=-1)
    xdt = xc * dtc[..., None]
    causal = jnp.tril(jnp.ones((SSM_CHUNK, SSM_CHUNK), dtype=bool))
    seg = a_cs[..., :, None] - a_cs[..., None, :]
    decay = jnp.exp(jnp.where(causal, seg, -jnp.inf))
    scores = jnp.einsum('bclhn,bcshn->bhcls', cc, bc) * decay
    y_diag = jnp.einsum('bhcls,bcshp->bclhp', scores, xdt)
    decay_states = jnp.exp(a_cs[..., -1:] - a_cs)
    states = jnp.einsum('bclhn,bhcl,bclhp->bchpn', bc, decay_states, xdt)
    chunk_decay = jnp.exp(a_cs[..., -1])

    def step(h, inp):
        s_c, d_c = inp
        return h * d_c[..., None, None] + s_c, h

    h0 = jnp.zeros((bsz, n_heads, hp, bmat.shape[-1]), jnp.float32)
    _, prev = lax.scan(step, h0, (states.transpose(1, 0, 2, 3, 4), chunk_decay.transpose(2, 0, 1)))
    prev = prev.transpose(1, 0, 2, 3, 4)
    y_off = jnp.einsum('bclhn,bchpn,bhcl->bclhp', cc, prev, jnp.exp(a_cs))
    y = (y_diag + y_off).reshape(bsz, n_chunk * SSM_CHUNK, n_heads, hp)
    return y[:, :seq]


def mamba2_branch(z, xs, bs, cs, dt_raw, conv_w, conv_b, dt_bias, a_log, d_skip, norm_w):
    bsz, seq, _ = z.shape
    xbc = jax.nn.silu(causal_dwconv(jnp.concatenate([xs, bs, cs], axis=-1), conv_w, conv_b))
    xs, bs, cs = jnp.split(xbc, [SSM_INNER, SSM_INNER + SSM_GROUPS * SSM_STATE], axis=-1)
    rep = SSM_HEADS // SSM_GROUPS
    xh = xs.reshape(bsz, seq, SSM_HEADS, SSM_HEAD_DIM).astype(jnp.float32)
    bh = jnp.repeat(bs.reshape(bsz, seq, SSM_GROUPS, SSM_STATE), rep, axis=2).astype(jnp.float32)
    ch = jnp.repeat(cs.reshape(bsz, seq, SSM_GROUPS, SSM_STATE), rep, axis=2).astype(jnp.float32)
    dt = jax.nn.softplus(dt_raw.astype(jnp.float32) + dt_bias.astype(jnp.float32))
    a = -jnp.exp(a_log.astype(jnp.float32))
    y = ssd_chunked(xh, dt, a, bh, ch) + d_skip.astype(jnp.float32)[:, None] * xh
    y = y.reshape(bsz, seq, SSM_INNER) * jax.nn.silu(z.astype(jnp.float32))
    yg = y.reshape(bsz, seq, SSM_GROUPS, -1)
    yg = yg * lax.rsqrt(jnp.mean(yg * yg, axis=-1, keepdims=True) + EPS)
    return (yg.reshape(bsz, seq, SSM_INNER) * norm_w.astype(jnp.float32)).astype(z.dtype)


def fox_branch(q, k, v, f_logit, f_bias):
    bsz, seq, n_heads, hd = q.shape
    log_f = jax.nn.log_sigmoid(f_logit.astype(jnp.float32) + f_bias.astype(jnp.float32))
    cf = jnp.cumsum(log_f, axis=1).transpose(0, 2, 1)
    kpos = jnp.arange(seq)
    scale = hd ** -0.5

    def block(i):
        start = i * Q_BLOCK
        qb = lax.dynamic_slice_in_dim(q, start, Q_BLOCK, axis=1)
        cq = lax.dynamic_slice_in_dim(cf, start, Q_BLOCK, axis=2)
        qpos = start + jnp.arange(Q_BLOCK)
        logits = jnp.einsum('bqhd,bkhd->bhqk', qb, k).astype(jnp.float32) * scale
        logits = logits + cq[..., :, None] - cf[..., None, :]
        logits = jnp.where(kpos[None, :] <= qpos[:, None], logits, -jnp.inf)
        p = jax.nn.softmax(logits, axis=-1).astype(v.dtype)
        return jnp.einsum('bhqk,bkhd->bqhd', p, v)

    out = lax.map(block, jnp.arange(seq // Q_BLOCK))
    return out.transpose(1, 0, 2, 3, 4).reshape(bsz, seq, n_heads * hd)


def moba_branch(q, k, v):
    bsz, seq, n_heads, hd = q.shape
    n_blk = -(-seq // MOBA_BLOCK)
    pad = n_blk * MOBA_BLOCK - seq
    to_blocks = lambda t: jnp.pad(t, ((0, 0), (0, pad), (0, 0), (0, 0))).reshape(
        bsz, n_blk, MOBA_BLOCK, n_heads, hd).transpose(0, 3, 1, 2, 4)
    k_blk, v_blk = to_blocks(k), to_blocks(v)
    k_mean = jnp.mean(k_blk.astype(jnp.float32), axis=3)
    n_sel = min(MOBA_TOPK, n_blk)
    n_sel_keys = n_sel * MOBA_BLOCK
    scale = hd ** -0.5
    b_ix = jnp.arange(bsz)[:, None, None, None]
    h_ix = jnp.arange(n_heads)[None, None, :, None]
    blk_ids = jnp.arange(n_blk)

    def block(i):
        start = i * MOBA_Q_BLOCK
        qb = lax.dynamic_slice_in_dim(q, start, MOBA_Q_BLOCK, axis=1)
        qpos = start + jnp.arange(MOBA_Q_BLOCK)
        cur = start // MOBA_BLOCK
        gate = jnp.einsum('bqhd,bhnd->bqhn', qb.astype(jnp.float32), k_mean)
        gate = jnp.where(blk_ids < cur, gate, -jnp.inf)
        _, sel = lax.top_k(gate, n_sel)
        sel_ok = sel < cur
        k_sel = k_blk[b_ix, h_ix, sel]
        v_sel = v_blk[b_ix, h_ix, sel]
        s_sel = jnp.einsum('bqhd,bqhnkd->bqhnk', qb, k_sel).astype(jnp.float32) * scale
        s_sel = jnp.where(sel_ok[..., None], s_sel, -jnp.inf)
        k_own = lax.dynamic_index_in_dim(k_blk, cur, axis=2, keepdims=False)
        v_own = lax.dynamic_index_in_dim(v_blk, cur, axis=2, keepdims=False)
        own_pos = cur * MOBA_BLOCK + jnp.arange(MOBA_BLOCK)
        s_own = jnp.einsum('bqhd,bhkd->bqhk', qb, k_own).astype(jnp.float32) * scale
        s_own = jnp.where(own_pos[None, None, None, :] <= qpos[None, :, None, None], s_own, -jnp.inf)
        logits = jnp.concatenate([s_sel.reshape(bsz, MOBA_Q_BLOCK, n_heads, n_sel_keys), s_own], axis=-1)
        p = jax.nn.softmax(logits, axis=-1).astype(v.dtype)
        p_sel = p[..., :n_sel_keys].reshape(bsz, MOBA_Q_BLOCK, n_heads, n_sel, MOBA_BLOCK)
        p_own = p[..., n_sel_keys:]
        return (jnp.einsum('bqhnk,bqhnkd->bqhd', p_sel, v_sel)
                + jnp.einsum('bqhk,bhkd->bqhd', p_own, v_own))

    out = lax.map(block, jnp.arange(seq // MOBA_Q_BLOCK))
    return out.transpose(1, 0, 2, 3, 4).reshape(bsz, seq, n_heads * hd)


def dsa_branch(q, k, v, qi, ki, wi):
    bsz, seq, n_heads, hd = q.shape
    n_keep = min(DSA_TOPK, seq // 4)
    kpos = jnp.arange(seq)
    scale = hd ** -0.5
    idx_scale = IDX_DIM ** -0.5
    w_scale = IDX_HEADS ** -0.5
    b_ix = jnp.arange(bsz)[:, None, None]

    def block(i):
        start = i * Q_BLOCK
        qb = lax.dynamic_slice_in_dim(q, start, Q_BLOCK, axis=1)
        qib = lax.dynamic_slice_in_dim(qi, start, Q_BLOCK, axis=1)
        wib = lax.dynamic_slice_in_dim(wi, start, Q_BLOCK, axis=1)
        qpos = start + jnp.arange(Q_BLOCK)
        isc = jax.nn.relu(jnp.einsum('bqhd,bkd->bqhk', qib, ki).astype(jnp.float32) * idx_scale)
        isc = jnp.einsum('bqh,bqhk->bqk', wib.astype(jnp.float32) * w_scale, isc)
        isc = jnp.where(kpos[None, None, :] <= qpos[None, :, None], isc, -jnp.inf)
        _, sel = lax.top_k(isc, n_keep)
        ok = sel <= qpos[None, :, None]
        k_sel = k[b_ix, sel]
        v_sel = v[b_ix, sel]
        logits = jnp.einsum('bqhd,bqkhd->bqhk', qb, k_sel).astype(jnp.float32) * scale
        logits = jnp.where(ok[:, :, None, :], logits, -jnp.inf)
        p = jax.nn.softmax(logits, axis=-1).astype(v.dtype)
        return jnp.einsum('bqhk,bqkhd->bqhd', p, v_sel)

    out = lax.map(block, jnp.arange(seq // Q_BLOCK))
    return out.transpose(1, 0, 2, 3, 4).reshape(bsz, seq, n_heads * hd)


def hybrid_mixer(h, w_in, conv_w, conv_b, dt_bias, a_log, d_skip, ssm_norm, fox_fbias,
                 w_gate, b_gate, w_branch, w_out, cos, sin):
    bsz, seq, _ = h.shape
    u = h @ w_in
    (z, xs, bs, cs, dt_raw, fq, fk, fv, ff, mq, mk, mv, dq, dk, dv, iq, ik, iw) = jnp.split(
        u, np.cumsum(IN_SPLITS)[:-1].tolist(), axis=-1)
    y_ssm = mamba2_branch(z, xs, bs, cs, dt_raw, conv_w, conv_b, dt_bias, a_log, d_skip, ssm_norm)
    y_fox = fox_branch(split_heads(fq, FOX_HEADS), split_heads(fk, FOX_HEADS),
                       split_heads(fv, FOX_HEADS), ff, fox_fbias)
    y_moba = moba_branch(apply_rope(split_heads(mq, MOBA_HEADS), cos, sin),
                         apply_rope(split_heads(mk, MOBA_HEADS), cos, sin),
                         split_heads(mv, MOBA_HEADS))
    y_dsa = dsa_branch(apply_rope(split_heads(dq, DSA_HEADS), cos, sin),
                       apply_rope(split_heads(dk, DSA_HEADS), cos, sin),
                       split_heads(dv, DSA_HEADS),
                       apply_rope(split_heads(iq, IDX_HEADS), cos, sin),
                       apply_rope(ik[:, :, None, :], cos, sin)[:, :, 0, :],
                       iw)
    ys = jnp.stack([y_ssm, y_fox, y_moba, y_dsa], axis=2)
    proj = jnp.einsum('bsnc,ncd->bsnd', ys, w_branch)
    gates = jax.nn.sigmoid(h @ w_gate + b_gate).reshape(bsz, seq, N_BRANCH, D_MODEL)
    merged = jnp.sum(gates * proj, axis=2)
    return merged @ w_out


def swiglu(h, w_in, w_out):
    g, up = jnp.split(h @ w_in, 2, axis=-1)
    return (jax.nn.silu(g) * up) @ w_out


def setup_inputs(seed: int = 0) -> dict:
    key = jax.random.key(seed)
    ks = jax.random.split(key, 19)
    f32 = jnp.float32
    nrm = lambda k, shape, scale: jax.random.normal(k, shape, f32) * scale
    gain = lambda k, shape: 1.0 + 0.05 * jax.random.normal(k, shape, f32)
    dt0 = jnp.exp(jax.random.uniform(ks[5], (DEPTH, SSM_HEADS), f32, math.log(1e-3), math.log(1e-1)))
    return {
        'x': nrm(ks[0], (BATCH, SEQ, D_MODEL), 1.0),
        'norm_mix_pre': gain(ks[1], (DEPTH, D_MODEL)),
        'w_in': nrm(ks[2], (DEPTH, D_MODEL, D_IN), D_MODEL ** -0.5),
        'conv_w': nrm(ks[3], (DEPTH, SSM_CONV, SSM_CONV_CH), SSM_CONV ** -0.5),
        'conv_b': nrm(ks[4], (DEPTH, SSM_CONV_CH), 0.02),
        'dt_bias': dt0 + jnp.log(-jnp.expm1(-dt0)),
        'a_log': jnp.log(jax.random.uniform(ks[6], (DEPTH, SSM_HEADS), f32, 1.0, 16.0)),
        'd_skip': 1.0 + 0.1 * jax.random.normal(ks[7], (DEPTH, SSM_HEADS), f32),
        'ssm_norm': gain(ks[8], (DEPTH, SSM_INNER)),
        'fox_fbias': 3.0 + 0.5 * jax.random.normal(ks[9], (DEPTH, FOX_HEADS), f32),
        'w_gate': nrm(ks[10], (DEPTH, D_MODEL, N_BRANCH * D_MODEL), D_MODEL ** -0.5),
        'b_gate': nrm(ks[11], (DEPTH, N_BRANCH * D_MODEL), 0.02),
        'w_branch': nrm(ks[12], (DEPTH, N_BRANCH, BRANCH_WIDTH, D_MODEL), BRANCH_WIDTH ** -0.5),
        'w_out': nrm(ks[13], (DEPTH, D_MODEL, D_MODEL), D_MODEL ** -0.5),
        'norm_mix_post': gain(ks[14], (DEPTH, D_MODEL)),
        'norm_ffn_pre': gain(ks[15], (DEPTH, D_MODEL)),
        'w_ffn_in': nrm(ks[16], (DEPTH, D_MODEL, 2 * FFN_HIDDEN), D_MODEL ** -0.5),
        'w_ffn_out': nrm(ks[17], (DEPTH, FFN_HIDDEN, D_MODEL), FFN_HIDDEN ** -0.5),
        'norm_ffn_post': gain(ks[18], (DEPTH, D_MODEL)),
    }


def reference(x, norm_mix_pre, w_in, conv_w, conv_b, dt_bias, a_log, d_skip, ssm_norm, fox_fbias,
              w_gate, b_gate, w_branch, w_out, norm_mix_post, norm_ffn_pre, w_ffn_in, w_ffn_out,
              norm_ffn_post):
    cos, sin = rope_tables(x.shape[1])
    for l in range(DEPTH):
        h = rms_norm(x, norm_mix_pre[l])
        mix = hybrid_mixer(h, w_in[l], conv_w[l], conv_b[l], dt_bias[l], a_log[l], d_skip[l],
                           ssm_norm[l], fox_fbias[l], w_gate[l], b_gate[l], w_branch[l], w_out[l],
                           cos, sin)
        x = x + rms_norm(mix, norm_mix_post[l])
        h = rms_norm(x, norm_ffn_pre[l])
        x = x + rms_norm(swiglu(h, w_ffn_in[l], w_ffn_out[l]), norm_ffn_post[l])
    return x
```

```python
import numpy as np
import ml_dtypes
import concourse.bass as bass
import concourse.mybir as mybir
from concourse.bass_utils import run_bass_kernel_spmd

F32 = mybir.dt.float32
BF16 = mybir.dt.bfloat16
AF = mybir.ActivationFunctionType
ALU = mybir.AluOpType
AX = mybir.AxisListType
NPBF = ml_dtypes.bfloat16

D = 1024
S = 8192
NB = 2
DEPTH = 2
TOWN = 2048
TT = 512
EPS = 1e-6
FFH = 2816
C_Z, C_XS, C_BS, C_CS, C_DT = 0, 512, 1024, 1280, 1536
C_FQ, C_FK, C_FV, C_FF = 1544, 2056, 2568, 3080
C_MQ, C_MK, C_MV = 3088, 3600, 4112
C_DQ, C_DK, C_DV = 4624, 5136, 5648
C_IQ, C_IK, C_IW = 6160, 6416, 6480
DIN = 6484
NEG = -1.0e30


class Buf:
    __slots__ = ("t", "name", "w", "r", "dsem", "dcnt")

    def __init__(self, t, name):
        self.t = t
        self.name = name
        self.w = None
        self.r = []
        self.dsem = None
        self.dcnt = 0

    def __getitem__(self, idx):
        return self.t[idx]


class KB:
    ENG = ("pe", "act", "dve", "pool", "sp")

    def __init__(self, nc):
        self.nc = nc
        self.eng = {"pe": nc.tensor, "act": nc.scalar, "dve": nc.vector,
                    "pool": nc.gpsimd, "sp": nc.sync}
        self.sem = {}
        self.cnt = {e: 0 for e in self.ENG}
        self._stack = []
        self._sems = []
        self.semobj = {}
        for e in self.ENG:
            g = nc.semaphore("s_" + e)
            self.sem[e] = g.__enter__()
            self._sems.append(g)
            self.semobj[e] = self.sem[e]
        self.waited = {e: {} for e in self.ENG}
        self.ndsem = 0
        self.dsems = {}
        self.rr = 0

    def sbuf(self, name, shape, dt=F32):
        g = self.nc.sbuf_tensor(name, list(shape), dt)
        t = g.__enter__()
        self._stack.append(g)
        return Buf(t, name)

    def psum(self, name, shape, dt=F32):
        g = self.nc.psum_tensor(name, list(shape), dt)
        t = g.__enter__()
        self._stack.append(g)
        return Buf(t, name)

    def dram(self, name, shape, dt=F32, kind="Internal"):
        t = self.nc.dram_tensor(name, list(shape), dt, kind=kind)
        return Buf(t.ap(), name)

    def close(self):
        for g in reversed(self._stack):
            g.__exit__(None, None, None)
        self._stack = []
        for g in reversed(self._sems):
            g.__exit__(None, None, None)
        self._sems = []

    def mark(self):
        return len(self._stack)

    def release(self, mark):
        self.barrier()
        while len(self._stack) > mark:
            self._stack.pop().__exit__(None, None, None)

    def _dsem(self, b):
        if b.dsem is None:
            g = self.nc.semaphore("d%d" % self.ndsem)
            b.dsem = g.__enter__()
            self._sems.append(g)
            self.ndsem += 1
            self.semobj[b.dsem.name] = b.dsem
        return b.dsem

    def share_dsem(self, bufs):
        ds = self._dsem(bufs[0])
        for b in bufs[1:]:
            b.dsem = ds

    def _wait(self, e, ev):
        if ev is None:
            return
        key, val = ev
        if self.waited[e].get(key, 0) >= val:
            return
        self.eng[e].wait_ge(self.semobj[key], val)
        self.waited[e][key] = val

    def _deps(self, e, reads, writes):
        for b in reads:
            self._wait(e, b.w)
        for b in writes:
            self._wait(e, b.w)
            for ev in b.r:
                self._wait(e, ev)

    def op(self, e, fn, reads=(), writes=()):
        self._deps(e, reads, writes)
        ins = fn(self.eng[e])
        self.cnt[e] += 1
        ins.then_inc(self.sem[e], 1)
        ev = (e, self.cnt[e])
        for b in reads:
            b.r.append(ev)
            if len(b.r) > 64:
                b.r = b.r[-64:] if False else b.r
        for b in writes:
            b.w = ev
            b.r = []
        return ins

    def dma(self, q, out, in_, reads=(), writes=(), **kw):
        self._deps(q, reads, writes)
        ins = self.eng[q].dma_start(out=out, in_=in_, **kw)
        tgt = writes[0]
        ds = self._dsem(tgt)
        cnts = self.dsems
        cnts[ds.name] = cnts.get(ds.name, 0) + 16
        ins.then_inc(ds, 16)
        ev = (ds.name, cnts[ds.name])
        for b in reads:
            b.r.append(ev)
        for b in writes:
            b.w = ev
            b.r = []
        return ins

    def finish(self, bufs, e="sp"):
        for b in bufs:
            self._wait(e, b.w)


def _prune(b):
    best = {}
    for key, val in b.r:
        if best.get(key, 0) < val:
            best[key] = val
    b.r = list(best.items())


_orig_op = KB.op


def _op(self, e, fn, reads=(), writes=()):
    ins = _orig_op(self, e, fn, reads, writes)
    for b in reads:
        if len(b.r) > 8:
            _prune(b)
    return ins


KB.op = _op


def emit_rstd(k, xT, ntok, ones_f, sq, ps, rstd):
    k.op("act", lambda e: e.activation(out=sq[:, :, :ntok], in_=xT[:, :, :ntok], func=AF.Square),
         reads=[xT], writes=[sq])
    for c in range(8):
        k.op("pe", lambda e, c=c: e.matmul(ps[:, :ntok], lhsT=ones_f[:], rhs=sq[:, c, :ntok],
                                            start=(c == 0), stop=(c == 7)),
             reads=[ones_f, sq], writes=[ps])
    k.op("act", lambda e: e.activation(out=rstd[:, :ntok], in_=ps[:, :ntok], func=AF.Sqrt,
                                       scale=1.0 / D, bias=EPS),
         reads=[ps], writes=[rstd])
    k.op("dve", lambda e: e.reciprocal(out=rstd[:, :ntok], in_=rstd[:, :ntok]), reads=[rstd], writes=[rstd])


def chunked(ap, p=128):
    return ap.rearrange("(c p) n -> p c n", p=p)


def build_A():
    nc = bass.Bass("TRN2", target_bir_lowering=False)
    k = KB(nc)
    EI, EO = "ExternalInput", "ExternalOutput"
    xT_d = k.dram("xT", [D, TOWN], F32, EI)
    gpre_d = k.dram("gpre", [128, 8], F32, EI)
    win_d = k.dram("w_in", [D, DIN], F32, EI)
    wsw_d = k.dram("w_sw", [D, DIN], F32, EI)
    tab_d = [k.dram(n, [128, TOWN], F32, EI) for n in ("cos2", "sinS", "cos2s", "sinSs")]
    fb_d = k.dram("fbias", [8, 1], F32, EI)

    hT_o = k.dram("hT", [D, TOWN], BF16, EO)
    zx_o = k.dram("zxbcT", [1536, TOWN], F32, EO)
    dt_o = k.dram("dtT", [8, TOWN], F32, EO)
    fm_o = {n: k.dram(n, [512, TOWN], BF16, EO) for n in ("fqT", "fkT", "mqT", "mkT", "dqT", "dkT")}
    fm_o["iqT"] = k.dram("iqT", [256, TOWN], BF16, EO)
    fm_o["ikT"] = k.dram("ikT", [64, TOWN], BF16, EO)
    tm_o = {n: k.dram(n, [TOWN, 512], BF16, EO) for n in ("fv", "mv", "dv")}
    lf_o = k.dram("logfT", [8, TOWN], F32, EO)
    iw_o = k.dram("iw", [TOWN, 4], F32, EO)
    km_o = k.dram("kmT", [512, 8], F32, EO)
    outs = [hT_o, zx_o, dt_o, lf_o, iw_o, km_o] + list(fm_o.values()) + list(tm_o.values())

    hT = k.sbuf("hT_s", [128, 8, TOWN], BF16)
    gpre = k.sbuf("gpre_s", [128, 8], F32)
    ones_f = k.sbuf("ones_f", [128, 128], F32)
    tabs = [k.sbuf("tab%d" % i, [128, TOWN], F32) for i in range(4)]
    fb = k.sbuf("fb_s", [8, 1], F32)
    nfb = k.sbuf("nfb_s", [8, 1], F32)
    xt = [k.sbuf("xt%d" % i, [128, 8, TT], F32) for i in range(2)]
    sq = k.sbuf("sq", [128, 8, TT], F32)
    rstd = k.sbuf("rstd", [128, TT], F32)
    ps_r = k.psum("ps_r", [128, TT], F32)
    psA = [k.psum("psA%d" % i, [128, TT], F32) for i in range(2)]
    psB = [k.psum("psB%d" % i, [128, TT], F32) for i in range(2)]
    wt = [k.sbuf("wt%d" % i, [128, 8, 128], BF16) for i in range(2)]
    ws = [k.sbuf("ws%d" % i, [128, 8, 128], BF16) for i in range(2)]
    wv = [k.sbuf("wv%d" % i, [128, 8, 512], BF16) for i in range(2)]
    stf = [k.sbuf("stf%d" % i, [128, TT], F32) for i in range(3)]
    stb = [k.sbuf("stb%d" % i, [128, TT], BF16) for i in range(3)]
    t1 = [k.sbuf("t1_%d" % i, [128, TT], F32) for i in range(2)]
    t2 = [k.sbuf("t2_%d" % i, [128, TT], F32) for i in range(2)]
    kms = k.sbuf("kms", [128, 4, 8], F32)
    iws = k.sbuf("iws", [128, 16, 4], F32)

    k.dma("sp", gpre[:], gpre_d[:], reads=[gpre_d], writes=[gpre])
    k.dma("sp", fb[:], fb_d[:], reads=[fb_d], writes=[fb])
    for i in range(4):
        k.dma("act", tabs[i][:], tab_d[i][:], reads=[tab_d[i]], writes=[tabs[i]])
    k.op("dve", lambda e: e.memset(ones_f[:], 1.0), writes=[ones_f])
    k.op("dve", lambda e: e.tensor_scalar(out=nfb[:], in0=fb[:], scalar1=-1.0, scalar2=None, op0=ALU.mult),
         reads=[fb], writes=[nfb])

    for tt in range(4):
        x_ = xt[tt % 2]
        tok = slice(tt * TT, (tt + 1) * TT)
        k.dma("sp", x_[:], chunked(xT_d[:, tok]), reads=[xT_d], writes=[x_])
        emit_rstd(k, x_, TT, ones_f, sq, ps_r, rstd)
        for c in range(8):
            k.op("dve", lambda e, c=c, x_=x_, tok=tok: e.scalar_tensor_tensor(
                out=hT[:, c, tok], in0=x_[:, c, :], scalar=gpre[:, c:c + 1], in1=rstd[:],
                op0=ALU.mult, op1=ALU.mult), reads=[x_, gpre, rstd], writes=[hT])
    k.dma("sp", chunked(hT_o[:]), hT[:], reads=[hT], writes=[hT_o])

    cnt = {"w": 0, "ps": 0, "st": 0, "t": 0, "wv": 0}

    def load_w(src_d, bufs, col0, ncols):
        w_ = bufs[cnt["w"] % 2]
        k.dma("pool", w_[:, :, :ncols], chunked(src_d[:, col0:col0 + ncols]), reads=[src_d], writes=[w_])
        return w_

    def mm_fm(ps, w_, ncols, tt):
        for c in range(8):
            k.op("pe", lambda e, c=c: e.matmul(ps[:ncols, :], lhsT=w_[:, c, :ncols],
                                                rhs=hT[:, c, tt * TT:(tt + 1) * TT],
                                                start=(c == 0), stop=(c == 7)),
                 reads=[w_, hT], writes=[ps])

    def fm_group(col0, ncols, kind, dst, row0, scale=1.0, tabi=0, kmchunk=None):
        w_ = load_w(win_d, wt, col0, ncols)
        if kind == "rope":
            w2 = load_w(wsw_d, ws, col0, ncols)
        cnt["w"] += 1
        for tt in range(4):
            tok = slice(tt * TT, (tt + 1) * TT)
            ps = psA[cnt["ps"] % 2]
            ps2 = psB[cnt["ps"] % 2]
            cnt["ps"] += 1
            mm_fm(ps, w_, ncols, tt)
            if kind == "f32":
                st = stf[cnt["st"] % 3]
                cnt["st"] += 1
                k.op("act", lambda e: e.activation(out=st[:ncols, :], in_=ps[:ncols, :], func=AF.Copy),
                     reads=[ps], writes=[st])
                k.dma("sp", dst[row0:row0 + ncols, tok], st[:ncols, :], reads=[st], writes=[dst])
            elif kind == "bf16":
                st = stb[cnt["st"] % 3]
                cnt["st"] += 1
                k.op("act", lambda e: e.activation(out=st[:ncols, :], in_=ps[:ncols, :], func=AF.Copy,
                                                   scale=scale), reads=[ps], writes=[st])
                k.dma("sp", dst[row0:row0 + ncols, tok], st[:ncols, :], reads=[st], writes=[dst])
            elif kind == "rope":
                mm_fm(ps2, w2, ncols, tt)
                a = t1[cnt["t"] % 2]
                b = t2[cnt["t"] % 2]
                cnt["t"] += 1
                ct, stt = tabs[tabi], tabs[tabi + 1]
                k.op("dve", lambda e: e.tensor_tensor(out=a[:ncols, :], in0=ps[:ncols, :], in1=ct[:ncols, tok],
                                                      op=ALU.mult), reads=[ps, ct], writes=[a])
                k.op("dve", lambda e: e.tensor_tensor(out=b[:ncols, :], in0=ps2[:ncols, :], in1=stt[:ncols, tok],
                                                      op=ALU.mult), reads=[ps2, stt], writes=[b])
                k.op("pool", lambda e: e.tensor_tensor(out=a[:ncols, :], in0=a[:ncols, :], in1=b[:ncols, :],
                                                       op=ALU.add), reads=[a, b], writes=[a])
                st = stb[cnt["st"] % 3]
                cnt["st"] += 1
                k.op("act", lambda e: e.activation(out=st[:ncols, :], in_=a[:ncols, :], func=AF.Copy),
                     reads=[a], writes=[st])
                k.dma("sp", dst[row0:row0 + ncols, tok], st[:ncols, :], reads=[st], writes=[dst])
                if kmchunk is not None:
                    k.op("dve", lambda e: e.tensor_reduce(
                        out=kms[:, kmchunk, 2 * tt:2 * tt + 2],
                        in_=a[:, :].rearrange("p (b t) -> p b t", b=2), axis=AX.X, op=ALU.add),
                        reads=[a], writes=[kms])
            elif kind == "logf":
                a = t1[cnt["t"] % 2]
                cnt["t"] += 1
                st = stf[cnt["st"] % 3]
                cnt["st"] += 1
                k.op("act", lambda e: e.activation(out=a[:8, :], in_=ps[:8, :], func=AF.Exp, scale=-1.0,
                                                   bias=nfb[:, 0:1]), reads=[ps, nfb], writes=[a])
                k.op("act", lambda e: e.activation(out=a[:8, :], in_=a[:8, :], func=AF.Ln, bias=1.0),
                     reads=[a], writes=[a])
                k.op("dve", lambda e: e.tensor_scalar(out=st[:8, :], in0=a[:8, :], scalar1=-1.0, scalar2=None,
                                                      op0=ALU.mult), reads=[a], writes=[st])
                k.dma("sp", dst[0:8, tok], st[:8, :], reads=[st], writes=[dst])

    def tm_group(col0, dst):
        w_ = wv[cnt["wv"] % 2]
        cnt["wv"] += 1
        k.dma("pool", w_[:], chunked(win_d[:, col0:col0 + 512]), reads=[win_d], writes=[w_])
        for t8 in range(16):
            ps = psA[cnt["ps"] % 2]
            cnt["ps"] += 1
            for c in range(8):
                k.op("pe", lambda e, c=c: e.matmul(ps[:, :], lhsT=hT[:, c, t8 * 128:(t8 + 1) * 128],
                                                    rhs=w_[:, c, :], start=(c == 0), stop=(c == 7)),
                     reads=[w_, hT], writes=[ps])
            st = stb[cnt["st"] % 3]
            cnt["st"] += 1
            if t8 % 2 == 0:
                k.op("act", lambda e: e.activation(out=st[:], in_=ps[:], func=AF.Copy), reads=[ps], writes=[st])
            else:
                k.op("dve", lambda e: e.tensor_copy(out=st[:], in_=ps[:]), reads=[ps], writes=[st])
            k.dma("sp", dst[t8 * 128:(t8 + 1) * 128, :], st[:], reads=[st], writes=[dst])

    for g in range(12):
        fm_group(C_Z + g * 128, 128, "f32", zx_o, g * 128)
    fm_group(C_DT, 8, "f32", dt_o, 0)
    for g in range(4):
        fm_group(C_FQ + g * 128, 128, "bf16", fm_o["fqT"], g * 128, scale=0.125)
    for g in range(4):
        fm_group(C_FK + g * 128, 128, "bf16", fm_o["fkT"], g * 128)
    fm_group(C_FF, 8, "logf", lf_o, 0)
    tm_group(C_FV, tm_o["fv"])
    for g in range(4):
        fm_group(C_MQ + g * 128, 128, "rope", fm_o["mqT"], g * 128, tabi=0)
    for g in range(4):
        fm_group(C_MK + g * 128, 128, "rope", fm_o["mkT"], g * 128, tabi=0, kmchunk=g)
    tm_group(C_MV, tm_o["mv"])
    for g in range(4):
        fm_group(C_DQ + g * 128, 128, "rope", fm_o["dqT"], g * 128, tabi=2)
    for g in range(4):
        fm_group(C_DK + g * 128, 128, "rope", fm_o["dkT"], g * 128, tabi=0)
    tm_group(C_DV, tm_o["dv"])
    for g in range(2):
        fm_group(C_IQ + g * 128, 128, "rope", fm_o["iqT"], g * 128, tabi=0)
    fm_group(C_IK, 64, "rope", fm_o["ikT"], 0, tabi=0)
    w_ = load_w(win_d, wt, C_IW, 4)
    cnt["w"] += 1
    for t8 in range(16):
        ps = psA[cnt["ps"] % 2]
        cnt["ps"] += 1
        for c in range(8):
            k.op("pe", lambda e, c=c: e.matmul(ps[:, :4], lhsT=hT[:, c, t8 * 128:(t8 + 1) * 128],
                                                rhs=w_[:, c, :4], start=(c == 0), stop=(c == 7)),
                 reads=[w_, hT], writes=[ps])
        k.op("act", lambda e: e.activation(out=iws[:, t8, :], in_=ps[:, :4], func=AF.Copy, scale=0.125 * 0.5),
             reads=[ps], writes=[iws])
    k.dma("sp", iw_o[:].rearrange("(t p) n -> p t n", p=128), iws[:], reads=[iws], writes=[iw_o])
    k.op("dve", lambda e: e.tensor_scalar(out=kms[:], in0=kms[:], scalar1=1.0 / 256, scalar2=None, op0=ALU.mult),
         reads=[kms], writes=[kms])
    k.dma("sp", chunked(km_o[:]), kms[:], reads=[kms], writes=[km_o])
    k.finish(outs, "sp")
    k.close()
    return nc


def own_positions(j):
    return np.concatenate([np.arange(512 * (4 * m + j), 512 * (4 * m + j) + 512) for m in range(4)])


def rope_tables_np(pos):
    inv = (10000.0 ** (-np.arange(0, 64, 2, dtype=np.float32) / 64)).astype(np.float32)
    ang = pos.astype(np.float32)[:, None] * inv[None, :]
    c, s = np.cos(ang).astype(np.float32), np.sin(ang).astype(np.float32)
    c64 = np.concatenate([c, c], 1).T
    s64 = np.concatenate([-s, s], 1).T
    cos2 = np.ascontiguousarray(np.concatenate([c64, c64], 0))
    sinS = np.ascontiguousarray(np.concatenate([s64, s64], 0))
    return cos2, sinS


def swap_halves_cols(w):
    n = w.shape[1]
    idx = np.arange(n).reshape(-1, 64)
    idx = np.concatenate([idx[:, 32:], idx[:, :32]], 1).reshape(-1)
    return w[:, idx]


def make_w_sw(w_in):
    w = np.array(w_in, copy=True)
    for c0, n in ((C_MQ, 512), (C_MK, 512), (C_DQ, 512), (C_DK, 512), (C_IQ, 256), (C_IK, 64)):
        w[:, c0:c0 + n] = swap_halves_cols(w_in[:, c0:c0 + n])
    return np.ascontiguousarray(w)


def barrier(k):
    evs = [(e, k.cnt[e]) for e in k.ENG if k.cnt[e] > 0] + [(n, v) for n, v in k.dsems.items()]
    for e in k.ENG:
        for ev in evs:
            if ev[0] != e:
                k._wait(e, ev)


KB.barrier = barrier
FILL = -2.0e30


def build_attn(kind):
    nc = bass.Bass("TRN2", target_bir_lowering=False)
    k = KB(nc)
    EI, EO = "ExternalInput", "ExternalOutput"
    KA = {"fox": 70, "moba": 96, "dsa": 64}[kind]
    qT_d = k.dram("qT", [512, TOWN], BF16, EI)
    kT_d = k.dram("kT", [512, S], BF16, EI)
    v_d = k.dram("v", [S, 512], BF16, EI)
    yT_o = k.dram("yT", [512, TOWN], BF16, EO)
    if kind != "dsa":
        dm_d = k.dram("diagm", [128, 16, 512], BF16, EI)
        diagm = k.sbuf("diagm_s", [128, 16, 512], BF16)
        k.dma("act", diagm[:], dm_d[:], reads=[dm_d], writes=[diagm])
    NBUF = 1 if kind == "dsa" else 2
    KT = [k.sbuf("KT%d" % i, [KA, S], BF16) for i in range(NBUF)]
    VT = [k.sbuf("VT%d" % i, [128, 64, 65], BF16) for i in range(NBUF)]
    QT = [k.sbuf("QT%d" % i, [KA, TT], BF16) for i in range(2)]
    pt = [k.sbuf("pt%d" % i, [128, TT], BF16) for i in range(3)]
    tmpf = [k.sbuf("tmpf%d" % i, [128, TT], F32) for i in range(2)]
    osb = k.sbuf("osb", [65, TT], F32)
    rec = k.sbuf("rec", [65, TT], F32)
    ones_r = k.sbuf("ones_r", [65, 64], F32)
    yst = [k.sbuf("yst%d" % i, [64, TT], BF16) for i in range(2)]
    ps = [k.psum("ps%d" % i, [128, TT], F32) for i in range(2)]
    po = [k.psum("po%d" % i, [65, TT], F32) for i in range(2)]
    pb = k.psum("pb", [64, TT], F32)
    k.op("dve", lambda e: e.memset(ones_r[:], 1.0), writes=[ones_r])
    for i in range(NBUF):
        k.op("pool", lambda e, i=i: e.memset(VT[i][:, :, 64:65], 1.0), writes=[VT[i]])

    if kind == "fox":
        lfa_d = k.dram("logf_all", [8, S], F32, EI)
        lfo_d = k.dram("logf_own", [8, TOWN], F32, EI)
        ind_d = k.dram("ind", [8, 4, 16], F32, EI)
        cfk_d = k.dram("cfk_scr", [8, 3, S], BF16)
        cfq_d = k.dram("cfq_scr", [8, 3, TOWN], BF16)
        for i in range(NBUF):
            k.op("dve", lambda e, i=i: e.memset(KT[i][64:70, :], 1.0), writes=[KT[i]])
        for i in range(2):
            k.op("dve", lambda e, i=i: e.memset(QT[i][64:70, :], 1.0), writes=[QT[i]])
        lf = k.sbuf("lf", [8, S], F32)
        cf = k.sbuf("cf", [8, S], F32)
        onesb = k.sbuf("onesb", [8, 4096], BF16)
        sp3 = k.sbuf("sp3", [8, 3, 4096], BF16)
        lfo = k.sbuf("lfo", [8, TOWN], F32)
        cfo = k.sbuf("cfo", [8, TOWN], F32)
        ind = k.sbuf("ind_s", [8, 4, 16], F32)
        tot = k.sbuf("tot", [8, 16], F32)
        tmp = k.sbuf("tmpi", [8, 4, 16], F32)
        bp = k.sbuf("bp", [8, 4], F32)
        k.dma("sp", lf[:], lfa_d[:], reads=[lfa_d], writes=[lf])
        k.dma("sp", lfo[:], lfo_d[:], reads=[lfo_d], writes=[lfo])
        k.dma("sp", ind[:], ind_d[:], reads=[ind_d], writes=[ind])
        k.op("pool", lambda e: e.memset(onesb[:], 1.0), writes=[onesb])
        k.op("dve", lambda e: e.tensor_tensor_scan(out=cf[:, :4096], data0=onesb[:], data1=lf[:, :4096],
                                                   initial=0.0, op0=ALU.mult, op1=ALU.add),
             reads=[onesb, lf], writes=[cf])
        k.op("dve", lambda e: e.tensor_tensor_scan(out=cf[:, 4096:], data0=onesb[:], data1=lf[:, 4096:],
                                                   initial=cf[:, 4095:4096], op0=ALU.mult, op1=ALU.add),
             reads=[onesb, lf, cf], writes=[cf])
        k.op("dve", lambda e: e.tensor_reduce(out=tot[:], in_=lf[:, :].rearrange("p (b t) -> p b t", b=16),
                                              axis=AX.X, op=ALU.add), reads=[lf], writes=[tot])
        for m in range(4):
            k.op("dve", lambda e, m=m: e.tensor_tensor(out=tmp[:, m, :], in0=ind[:, m, :], in1=tot[:],
                                                       op=ALU.mult), reads=[ind, tot], writes=[tmp])
        k.op("dve", lambda e: e.tensor_reduce(out=bp[:], in_=tmp[:], axis=AX.X, op=ALU.add),
             reads=[tmp], writes=[bp])
        for m in range(4):
            k.op("dve", lambda e, m=m: e.tensor_tensor_scan(
                out=cfo[:, m * 512:(m + 1) * 512], data0=onesb[:, :512], data1=lfo[:, m * 512:(m + 1) * 512],
                initial=bp[:, m:m + 1], op0=ALU.mult, op1=ALU.add), reads=[onesb, lfo, bp], writes=[cfo])

        def split3(src, n, dst_d, sign):
            if sign < 0:
                k.op("dve", lambda e: e.tensor_scalar(out=src[:, :n], in0=src[:, :n], scalar1=-1.0, scalar2=None,
                                                      op0=ALU.mult), reads=[src], writes=[src])
            for c0 in range(0, n, 4096):
                w = min(4096, n - c0)
                cs = slice(c0, c0 + w)
                for i in range(3):
                    k.op("dve", lambda e, i=i: e.tensor_copy(out=sp3[:, i, :w], in_=src[:, cs]),
                         reads=[src], writes=[sp3])
                    if i < 2:
                        k.op("dve", lambda e, i=i: e.tensor_tensor(out=src[:, cs], in0=src[:, cs],
                                                                   in1=sp3[:, i, :w], op=ALU.subtract),
                             reads=[src, sp3], writes=[src])
                k.dma("sp", dst_d[:, :, cs], sp3[:, :, :w], reads=[sp3], writes=[dst_d])

        split3(cf, S, cfk_d, -1.0)
        split3(cfo, TOWN, cfq_d, 1.0)
    if kind == "moba":
        oh_d = k.dram("onehot", [32, S], BF16, EI)
        km_d = k.dram("kmT_all", [512, 32], F32, EI)
        tb_d = [k.dram(n, [128, 16, 32], F32, EI) for n in ("validneg", "valid01", "own01")]
        tb = [k.sbuf("mtb%d" % i, [128, 16, 32], F32) for i in range(3)]
        for i in range(3):
            k.dma("act", tb[i][:], tb_d[i][:], reads=[tb_d[i]], writes=[tb[i]])
        for i in range(NBUF):
            k.dma("act", KT[i][64:96, :], oh_d[:], reads=[oh_d], writes=[KT[i]])
        kmf = k.sbuf("kmf", [64, 8, 32], F32)
        kmb = k.sbuf("kmb", [64, 8, 32], BF16)
        k.dma("sp", kmf[:], km_d[:].rearrange("(h d) n -> d h n", d=64), reads=[km_d], writes=[kmf])
        k.op("dve", lambda e: e.tensor_copy(out=kmb[:], in_=kmf[:]), reads=[kmf], writes=[kmb])
        identb = k.sbuf("identb", [128, 128], BF16)
        id_d = k.dram("ident", [128, 128], BF16, EI)
        k.dma("sp", identb[:], id_d[:], reads=[id_d], writes=[identb])
        nmw = k.sbuf("nmw", [128, 96], BF16)
        k.op("dve", lambda e: e.memset(nmw[:], 0.0), writes=[nmw])
        gm = k.sbuf("gm", [128, 32], F32)
        m8 = k.sbuf("m8", [128, 8], F32)
        sel = k.sbuf("sel", [128, 32], F32)
        pg = k.psum("pg", [128, 32], F32)
        pn = k.psum("pn", [96, 128], F32)
    if kind == "dsa":
        iq_d = k.dram("iqT", [256, TOWN], BF16, EI)
        ik_d = k.dram("ikT_all", [64, S], BF16, EI)
        iw_d = k.dram("iw", [TOWN, 4], F32, EI)
        dn_d = k.dram("diagneg", [128, 4, 2048], BF16, EI)
        id_d = k.dram("identf", [128, 128], F32, EI)
        iq = k.sbuf("iq_s", [64, 4, TOWN], BF16)
        ik = k.sbuf("ik_s", [64, S], BF16)
        iw = k.sbuf("iw_s", [128, 16, 4], F32)
        dneg = k.sbuf("dneg", [128, 4, 2048], BF16)
        identf = k.sbuf("identf_s", [128, 128], F32)
        k.dma("sp", iq[:], iq_d[:].rearrange("(h d) t -> d h t", d=64), reads=[iq_d], writes=[iq])
        k.dma("sp", ik[:], ik_d[:], reads=[ik_d], writes=[ik])
        k.dma("sp", iw[:], iw_d[:].rearrange("(t p) n -> p t n", p=128), reads=[iw_d], writes=[iw])
        k.dma("act", dneg[:], dn_d[:], reads=[dn_d], writes=[dneg])
        k.dma("act", identf[:], id_d[:], reads=[id_d], writes=[identf])
        isc = k.sbuf("isc", [128, S], F32)
        maskT = k.sbuf("maskT", [128, 64, TT], BF16)
        rl = [k.sbuf("rl%d" % i, [128, TT], F32) for i in range(2)]
        m8 = k.sbuf("m8", [128, 8], F32)
        pi = [k.psum("pi%d" % i, [128, TT], F32) for i in range(2)]
        ptr = k.psum("ptr", [128, 4, 128], F32)

    c = {"kv": 0, "q": 0, "ps": 0, "pt": 0, "po": 0, "y": 0, "rl": 0}

    for m in range(4):
        L = 2048 * (m + 1)
        nkt = L // 128
        qs = slice(m * TT, (m + 1) * TT)
        if kind == "dsa":
            for sub in range(4):
                st = m * 4 + sub
                q128 = slice(m * TT + sub * 128, m * TT + (sub + 1) * 128)
                for kc in range(L // 512):
                    ksl = slice(kc * 512, (kc + 1) * 512)
                    for hi in range(4):
                        p_ = pi[c["ps"] % 2]
                        r_ = rl[c["rl"] % 2]
                        c["ps"] += 1
                        c["rl"] += 1
                        k.op("pe", lambda e: e.matmul(p_[:], lhsT=iq[:, hi, q128], rhs=ik[:, ksl],
                                                      start=True, stop=True), reads=[iq, ik], writes=[p_])
                        k.op("act", lambda e: e.activation(out=r_[:], in_=p_[:], func=AF.Relu),
                             reads=[p_], writes=[r_])
                        if hi == 0:
                            k.op("dve", lambda e: e.tensor_scalar(out=isc[:, ksl], in0=r_[:],
                                                                  scalar1=iw[:, st, 0:1], scalar2=None,
                                                                  op0=ALU.mult), reads=[r_, iw], writes=[isc])
                        else:
                            k.op("dve", lambda e: e.scalar_tensor_tensor(
                                out=isc[:, ksl], in0=r_[:], scalar=iw[:, st, hi:hi + 1], in1=isc[:, ksl],
                                op0=ALU.mult, op1=ALU.add), reads=[r_, iw, isc], writes=[isc])
                dsl = slice(L - 2048, L)
                k.op("dve", lambda e: e.tensor_tensor(out=isc[:, dsl], in0=isc[:, dsl], in1=dneg[:, sub, :],
                                                      op=ALU.add), reads=[isc, dneg], writes=[isc])
                for r in range(32):
                    k.op("dve", lambda e: e.max(out=m8[:], in_=isc[:, :L]), reads=[isc], writes=[m8])
                    k.op("dve", lambda e: e.match_replace(out=isc[:, :L], in_to_replace=m8[:],
                                                          in_values=isc[:, :L], imm_value=FILL),
                         reads=[isc, m8], writes=[isc])
                k.op("dve", lambda e: e.tensor_scalar(out=isc[:, :L], in0=isc[:, :L], scalar1=FILL, scalar2=None,
                                                      op0=ALU.is_equal), reads=[isc], writes=[isc])
                k.op("dve", lambda e: e.scalar_tensor_tensor(
                    out=isc[:, dsl], in0=dneg[:, sub, :], scalar=-1.0, in1=isc[:, dsl],
                    op0=ALU.is_gt, op1=ALU.mult), reads=[isc, dneg], writes=[isc])
                for k4 in range(nkt // 4):
                    for i in range(4):
                        kt = k4 * 4 + i
                        k.op("pe", lambda e, i=i, kt=kt: e.transpose(out=ptr[:, i, :],
                                                                     in_=isc[:, kt * 128:(kt + 1) * 128],
                                                                     identity=identf[:]),
                             reads=[isc, identf], writes=[ptr])
                    k.op("act", lambda e, k4=k4: e.activation(
                        out=maskT[:, k4 * 4:k4 * 4 + 4, sub * 128:(sub + 1) * 128], in_=ptr[:], func=AF.Copy),
                        reads=[ptr], writes=[maskT])
        for h in range(8):
            K_ = KT[c["kv"] % NBUF]
            V_ = VT[c["kv"] % NBUF]
            c["kv"] += 1
            Q_ = QT[c["q"] % 2]
            c["q"] += 1
            hs = slice(h * 64, (h + 1) * 64)
            k.dma("sp", K_[0:64, :L], kT_d[hs, :L], reads=[kT_d], writes=[K_])
            k.dma("act", V_[:, :nkt, 0:64], v_d[:L, hs].rearrange("(t p) d -> p t d", p=128),
                  reads=[v_d], writes=[V_])
            k.dma("sp", Q_[0:64, :], qT_d[hs, qs], reads=[qT_d], writes=[Q_])
            if kind == "fox":
                k.dma("sp", K_[64:67, :L], cfk_d[h, :, :L], reads=[cfk_d], writes=[K_])
                k.dma("sp", Q_[67:70, :], cfq_d[h, :, qs], reads=[cfq_d], writes=[Q_])
            if kind == "moba":
                for sub in range(4):
                    st = m * 4 + sub
                    k.op("pe", lambda e: e.matmul(pg[:], lhsT=Q_[0:64, sub * 128:(sub + 1) * 128],
                                                  rhs=kmb[:, h, :], start=True, stop=True),
                         reads=[Q_, kmb], writes=[pg])
                    k.op("dve", lambda e: e.tensor_tensor(out=gm[:], in0=pg[:], in1=tb[0][:, st, :], op=ALU.add),
                         reads=[pg, tb[0]], writes=[gm])
                    k.op("dve", lambda e: e.max(out=m8[:], in_=gm[:]), reads=[gm], writes=[m8])
                    k.op("dve", lambda e: e.tensor_scalar(out=sel[:], in0=gm[:], scalar1=m8[:, 2:3], scalar2=None,
                                                          op0=ALU.is_ge), reads=[gm, m8], writes=[sel])
                    k.op("dve", lambda e: e.tensor_tensor(out=sel[:], in0=sel[:], in1=tb[1][:, st, :],
                                                          op=ALU.mult), reads=[sel, tb[1]], writes=[sel])
                    k.op("dve", lambda e: e.tensor_tensor(out=sel[:], in0=sel[:], in1=tb[2][:, st, :],
                                                          op=ALU.add), reads=[sel, tb[2]], writes=[sel])
                    k.op("dve", lambda e: e.tensor_scalar(out=nmw[:, 64:96], in0=sel[:], scalar1=-1.0,
                                                          scalar2=30000.0, op0=ALU.add, op1=ALU.mult),
                         reads=[sel], writes=[nmw])
                    k.op("pe", lambda e: e.matmul(pn[:], lhsT=nmw[:], rhs=identb[:], start=True, stop=True),
                         reads=[nmw, identb], writes=[pn])
                    k.op("act", lambda e: e.activation(out=Q_[64:96, sub * 128:(sub + 1) * 128],
                                                       in_=pn[64:96, :], func=AF.Copy), reads=[pn], writes=[Q_])
            o_ = po[c["po"] % 2]
            c["po"] += 1
            for kt in range(nkt):
                s_ = ps[c["ps"] % 2]
                c["ps"] += 1
                p_ = pt[c["pt"] % 3]
                c["pt"] += 1
                k.op("pe", lambda e: e.matmul(s_[:], lhsT=K_[0:KA, kt * 128:(kt + 1) * 128], rhs=Q_[0:KA, :],
                                              start=True, stop=True), reads=[K_, Q_], writes=[s_])
                esc = (0.125 if kind == "moba" else 1.0)
                if kind != "dsa" and kt >= 16 * m:
                    t_ = tmpf[c["pt"] % 2]
                    k.op("dve", lambda e: e.tensor_tensor(out=t_[:], in0=s_[:], in1=diagm[:, kt - 16 * m, :],
                                                          op=ALU.add), reads=[s_, diagm], writes=[t_])
                    k.op("act", lambda e: e.activation(out=p_[:], in_=t_[:], func=AF.Exp, scale=esc),
                         reads=[t_], writes=[p_])
                else:
                    k.op("act", lambda e: e.activation(out=p_[:], in_=s_[:], func=AF.Exp, scale=esc),
                         reads=[s_], writes=[p_])
                if kind == "dsa":
                    k.op("pool" if kt % 2 else "dve",
                         lambda e: e.tensor_tensor(out=p_[:], in0=p_[:], in1=maskT[:, kt, :], op=ALU.mult),
                         reads=[p_, maskT], writes=[p_])
                k.op("pe", lambda e: e.matmul(o_[:], lhsT=V_[:, kt, :], rhs=p_[:], start=(kt == 0),
                                              stop=(kt == nkt - 1)), reads=[V_, p_], writes=[o_])
            k.op("act", lambda e: e.activation(out=osb[:], in_=o_[:], func=AF.Copy), reads=[o_], writes=[osb])
            k.op("dve", lambda e: e.reciprocal(out=rec[64:65, :], in_=osb[64:65, :]), reads=[osb], writes=[rec])
            k.op("pe", lambda e: e.matmul(pb[:], lhsT=ones_r[64:65, :], rhs=rec[64:65, :], start=True, stop=True),
                 reads=[ones_r, rec], writes=[pb])
            y_ = yst[c["y"] % 2]
            c["y"] += 1
            k.op("dve", lambda e: e.tensor_tensor(out=y_[:], in0=osb[0:64, :], in1=pb[:], op=ALU.mult),
                 reads=[osb, pb], writes=[y_])
            k.dma("sp", yT_o[hs, qs], y_[:], reads=[y_], writes=[yT_o])
    k.finish([yT_o], "sp")
    k.close()
    return nc


def diag_tables(j):
    kp = np.arange(2048)
    qp = j * 512 + np.arange(512)
    vis = (kp[:, None] <= qp[None, :])
    diagm = np.where(vis, 0.0, -30000.0).reshape(16, 128, 512).transpose(1, 0, 2).astype(NPBF)
    dn = np.where(vis.T, 0.0, NEG).astype(np.float32)
    diagneg = dn.reshape(4, 128, 2048).transpose(1, 0, 2).astype(NPBF)
    return np.ascontiguousarray(diagm), np.ascontiguousarray(diagneg)


def moba_tables(j):
    pos = own_positions(j).reshape(16, 128).T
    cur = pos // 256
    n = np.arange(32)[None, None, :]
    valid = n < cur[:, :, None]
    own = n == cur[:, :, None]
    return (np.where(valid, 0.0, NEG).astype(np.float32), valid.astype(np.float32), own.astype(np.float32))


def gather_fm(res, name, b):
    r0 = np.asarray(res[b * 4][name])
    out = np.empty((r0.shape[0], S), r0.dtype)
    for j in range(4):
        out[:, own_positions(j)] = np.asarray(res[b * 4 + j][name])
    return out


def gather_tm(res, name, b):
    r0 = np.asarray(res[b * 4][name])
    out = np.empty((S, r0.shape[1]), r0.dtype)
    for j in range(4):
        out[own_positions(j)] = np.asarray(res[b * 4 + j][name])
    return out


def run(nc, maps):
    return run_bass_kernel_spmd(nc, maps, core_ids=list(range(8))).results


def build_ssd():
    nc = bass.Bass("TRN2", target_bir_lowering=False)
    k = KB(nc)
    EI, EO = "ExternalInput", "ExternalOutput"
    pre_d = [k.dram(n, [128, S], F32, EI) for n in ("xpre", "Bpre", "Cpre")]
    z_d = k.dram("ztok", [S, 128], F32, EI)
    dtr_d = k.dram("dtraw", [S, 2], F32, EI)
    cw_d = k.dram("convw", [128, 3, 4], F32, EI)
    cb_d = k.dram("convb", [128, 3], F32, EI)
    par_d = k.dram("par", [128, 3, 2], F32, EI)
    tri_d = k.dram("tri", [128, 2, 256], F32, EI)
    ntri_d = k.dram("negtri", [128, 2, 256], F32, EI)
    id_d = k.dram("identf", [128, 128], F32, EI)
    y_o = k.dram("y", [S, 128], F32, EO)

    cw = k.sbuf("cw_sb", [128, 3, 4], F32)
    cb = k.sbuf("cb_sb", [128, 3], F32)
    par = k.sbuf("par_sb", [128, 3, 2], F32)
    tri = k.sbuf("tri_sb", [128, 2, 256], F32)
    ntri = k.sbuf("ntri_sb", [128, 2, 256], F32)
    identf = k.sbuf("identf_sb", [128, 128], F32)
    ones_f = k.sbuf("ones_f_sb", [128, 128], F32)
    for dst, src in ((cw, cw_d), (cb, cb_d), (par, par_d), (tri, tri_d), (ntri, ntri_d), (identf, id_d)):
        k.dma("sp", dst[:], src[:], reads=[src], writes=[dst])
    k.op("dve", lambda e: e.memset(ones_f[:], 1.0), writes=[ones_f])
    dt = k.sbuf("dt_sb", [128, 64, 2], F32)
    dta = k.sbuf("dta_sb", [128, 64, 2], F32)
    aneg = k.sbuf("aneg", [128, 2], F32)
    k.dma("sp", dt[:], dtr_d[:].rearrange("(t p) h -> p t h", p=128), reads=[dtr_d], writes=[dt])
    for hh in range(2):
        k.op("dve", lambda e, hh=hh: e.tensor_scalar(out=dt[:, :, hh], in0=dt[:, :, hh], scalar1=par[:, 0, hh:hh + 1],
                                                     scalar2=None, op0=ALU.add), reads=[dt, par], writes=[dt])
    k.op("act", lambda e: e.activation(out=dt[:], in_=dt[:], func=AF.Exp), reads=[dt], writes=[dt])
    k.op("act", lambda e: e.activation(out=dt[:], in_=dt[:], func=AF.Ln, bias=1.0), reads=[dt], writes=[dt])
    k.op("act", lambda e: e.activation(out=aneg[:], in_=par[:, 1, :], func=AF.Exp), reads=[par], writes=[aneg])
    k.op("dve", lambda e: e.tensor_scalar(out=aneg[:], in0=aneg[:], scalar1=-1.0, scalar2=None, op0=ALU.mult),
         reads=[aneg], writes=[aneg])
    for hh in range(2):
        k.op("dve", lambda e, hh=hh: e.tensor_scalar(out=dta[:, :, hh], in0=dt[:, :, hh], scalar1=aneg[:, hh:hh + 1],
                                                     scalar2=None, op0=ALU.mult), reads=[dt, aneg], writes=[dta])

    SEG = 2048
    pre = k.sbuf("pre", [128, SEG + 3], F32)
    acc = k.sbuf("acc", [128, SEG], F32)
    xTc = k.sbuf("xTc", [128, SEG], F32)
    xtok = k.sbuf("xtok", [128, 16, 128], F32)
    BTb = k.sbuf("BTb", [128, SEG], BF16)
    Btok = k.sbuf("Btok", [128, 16, 128], BF16)
    CTf = k.sbuf("CTf", [128, SEG], F32)
    CTb = k.sbuf("CTb", [128, SEG], BF16)
    ztok = k.sbuf("ztok_sb", [128, 16, 128], F32)
    ptr = k.psum("ptr", [128, 4, 128], F32)
    pc = k.psum("pc", [128, 2, 2], F32)
    pbc = k.psum("pbc", [128, 2, 256], F32)
    pG = k.psum("pG", [128, 2, 256], F32)
    py = k.psum("py", [128, 2, 128], F32)
    pst = k.psum("pst", [128, 2, 64], F32)
    acs = k.sbuf("acs", [128, 2, 2], F32)
    nacs = k.sbuf("nacs", [128, 2, 2], F32)
    dtab = k.sbuf("dtab", [128, 2, 2, 128], F32)
    E = k.sbuf("E", [128, 2, 256], F32)
    bce = k.sbuf("bce", [128, 2], F32)
    cd = k.sbuf("cd", [128, 2], F32)
    tmpd = [k.sbuf("tmpd%d" % i, [128, 256], F32) for i in range(2)]
    dec = [k.sbuf("dec%d" % i, [128, 256], F32) for i in range(2)]
    sc = k.sbuf("sc", [128, 2, 2, 256], BF16)
    Cp = k.sbuf("Cp", [128, 2, 256], BF16)
    xdt = k.sbuf("xdt", [128, 2, 2, 64], BF16)
    xdp = k.sbuf("xdp", [128, 2, 2, 64], BF16)
    decl = k.sbuf("decl", [128, 2, 2], F32)
    wl = k.sbuf("wl", [128, 2, 2], F32)
    hst = k.sbuf("hst", [128, 2, 64], F32)
    hb = k.sbuf("hb", [128, 2, 64], BF16)
    yo = [k.sbuf("yo%d" % i, [128, 128], F32) for i in range(2)]
    sz = [k.sbuf("sz%d" % i, [128, 128], F32) for i in range(2)]
    k.op("dve", lambda e: e.memset(hst[:], 0.0), writes=[hst])
    k.op("dve", lambda e: e.memset(hb[:], 0.0), writes=[hb])

    def conv_silu(ci, seg, out_f32):
        src = pre_d[ci]
        if seg == 0:
            k.op("dve", lambda e: e.memset(pre[:, 0:3], 0.0), writes=[pre])
            k.dma("sp", pre[:, 3:], src[:, 0:SEG], reads=[src], writes=[pre])
        else:
            k.dma("sp", pre[:], src[:, seg * SEG - 3:(seg + 1) * SEG], reads=[src], writes=[pre])
        k.op("dve", lambda e: e.tensor_scalar(out=acc[:], in0=pre[:, 0:SEG], scalar1=cw[:, ci, 0:1], scalar2=None,
                                              op0=ALU.mult), reads=[pre, cw], writes=[acc])
        for w in range(1, 4):
            k.op("dve", lambda e, w=w: e.scalar_tensor_tensor(out=acc[:], in0=pre[:, w:w + SEG],
                                                               scalar=cw[:, ci, w:w + 1], in1=acc[:],
                                                               op0=ALU.mult, op1=ALU.add),
                 reads=[pre, cw, acc], writes=[acc])
        k.op("act", lambda e: e.activation(out=out_f32[:], in_=acc[:], func=AF.Silu, bias=cb[:, ci:ci + 1]),
             reads=[acc, cb], writes=[out_f32])

    def to_tok(srcT, dst):
        for t4 in range(4):
            for i in range(4):
                t = t4 * 4 + i
                k.op("pe", lambda e, i=i, t=t: e.transpose(out=ptr[:, i, :], in_=srcT[:, t * 128:(t + 1) * 128],
                                                           identity=identf[:]), reads=[srcT, identf], writes=[ptr])
            k.op("act", lambda e, t4=t4: e.activation(out=dst[:, t4 * 4:t4 * 4 + 4, :], in_=ptr[:], func=AF.Copy),
                 reads=[ptr], writes=[dst])

    for seg in range(S // SEG):
        conv_silu(0, seg, xTc)
        to_tok(xTc, xtok)
        conv_silu(1, seg, CTf)
        k.op("dve", lambda e: e.tensor_copy(out=BTb[:], in_=CTf[:]), reads=[CTf], writes=[BTb])
        to_tok(CTf, Btok)
        conv_silu(2, seg, CTf)
        k.op("dve", lambda e: e.tensor_copy(out=CTb[:], in_=CTf[:]), reads=[CTf], writes=[CTb])
        k.dma("act", ztok[:], z_d[seg * SEG:(seg + 1) * SEG, :].rearrange("(t p) c -> p t c", p=128),
              reads=[z_d], writes=[ztok])
        for cl in range(SEG // 256):
            t0 = seg * 16 + cl * 2
            lt = cl * 2
            csl = slice(cl * 256, (cl + 1) * 256)
            k.op("pe", lambda e: e.matmul(pc[:, 0, :], lhsT=tri[:, 0, 0:128], rhs=dta[:, t0, :], start=True, stop=True),
                 reads=[tri, dta], writes=[pc])
            k.op("pe", lambda e: e.matmul(pc[:, 1, :], lhsT=ones_f[:], rhs=dta[:, t0, :], start=True, stop=False),
                 reads=[ones_f, dta], writes=[pc])
            k.op("pe", lambda e: e.matmul(pc[:, 1, :], lhsT=tri[:, 0, 0:128], rhs=dta[:, t0 + 1, :], start=False,
                                          stop=True), reads=[tri, dta], writes=[pc])
            k.op("act", lambda e: e.activation(out=acs[:], in_=pc[:], func=AF.Copy), reads=[pc], writes=[acs])
            k.op("dve", lambda e: e.tensor_scalar(out=nacs[:], in0=pc[:], scalar1=-1.0, scalar2=None, op0=ALU.mult),
                 reads=[pc], writes=[nacs])
            for i in range(2):
                for h in range(2):
                    k.op("pool", lambda e, i=i, h=h: e.tensor_copy(
                        out=dtab[:, i, h, :], in_=dta[:, t0 + i, h:h + 1].to_broadcast([128, 128])),
                        reads=[dta], writes=[dtab])
            for h in range(2):
                for i in range(2):
                    k.op("pe", lambda e, i=i, h=h: e.matmul(pbc[:, h, :], lhsT=dtab[:, i, h, :], rhs=tri[:, i, :],
                                                            start=(i == 0), stop=(i == 1)),
                         reads=[dtab, tri], writes=[pbc])
            k.op("act", lambda e: e.activation(out=E[:], in_=pbc[:], func=AF.Exp), reads=[pbc], writes=[E])
            k.op("act", lambda e: e.activation(out=bce[:], in_=pbc[:, :, 255], func=AF.Copy), reads=[pbc], writes=[bce])
            k.op("act", lambda e: e.activation(out=cd[:], in_=bce[:], func=AF.Exp), reads=[bce], writes=[cd])
            for i in range(2):
                k.op("pe", lambda e, i=i: e.matmul(pG[:, i, :], lhsT=BTb[:, (lt + i) * 128:(lt + i + 1) * 128],
                                                   rhs=CTb[:, csl], start=True, stop=True),
                     reads=[BTb, CTb], writes=[pG])
            for h in range(2):
                for i in range(2):
                    t_ = tmpd[(h * 2 + i) % 2]
                    d_ = dec[(h * 2 + i) % 2]
                    k.op("dve", lambda e: e.tensor_tensor(out=t_[:], in0=pbc[:, h, :], in1=ntri[:, i, :], op=ALU.add),
                         reads=[pbc, ntri], writes=[t_])
                    k.op("act", lambda e: e.activation(out=d_[:], in_=t_[:], func=AF.Exp, bias=nacs[:, i, h:h + 1]),
                         reads=[t_, nacs], writes=[d_])
                    k.op("dve", lambda e: e.tensor_tensor(out=sc[:, h, i, :], in0=pG[:, i, :], in1=d_[:], op=ALU.mult),
                         reads=[pG, d_], writes=[sc])
                k.op("pool", lambda e: e.tensor_tensor(out=Cp[:, h, :], in0=CTf[:, csl], in1=E[:, h, :], op=ALU.mult),
                     reads=[CTf, E], writes=[Cp])
                for i in range(2):
                    hs = slice(h * 64, (h + 1) * 64)
                    k.op("dve", lambda e: e.tensor_scalar(out=xdt[:, h, i, :], in0=xtok[:, lt + i, hs],
                                                          scalar1=dt[:, t0 + i, h:h + 1], scalar2=None, op0=ALU.mult),
                         reads=[xtok, dt], writes=[xdt])
                    k.op("act", lambda e: e.activation(out=decl[:, h, i:i + 1], in_=acs[:, i, h:h + 1], func=AF.Exp,
                                                       scale=-1.0, bias=bce[:, h:h + 1]),
                         reads=[acs, bce], writes=[decl])
                    k.op("dve", lambda e: e.tensor_tensor(out=wl[:, h, i:i + 1], in0=decl[:, h, i:i + 1],
                                                          in1=dt[:, t0 + i, h:h + 1], op=ALU.mult),
                         reads=[decl, dt], writes=[wl])
                    k.op("dve", lambda e: e.tensor_scalar(out=xdp[:, h, i, :], in0=xtok[:, lt + i, hs],
                                                          scalar1=wl[:, h, i:i + 1], scalar2=None, op0=ALU.mult),
                         reads=[xtok, wl], writes=[xdp])
            for li in range(2):
                for h in range(2):
                    hs = slice(h * 64, (h + 1) * 64)
                    k.op("pe", lambda e: e.matmul(py[:, li, hs], lhsT=sc[:, h, 0, li * 128:(li + 1) * 128],
                                                  rhs=xdt[:, h, 0, :], start=True, stop=False),
                         reads=[sc, xdt], writes=[py])
                    if li == 1:
                        k.op("pe", lambda e: e.matmul(py[:, li, hs], lhsT=sc[:, h, 1, 128:256], rhs=xdt[:, h, 1, :],
                                                      start=False, stop=False), reads=[sc, xdt], writes=[py])
                    k.op("pe", lambda e: e.matmul(py[:, li, hs], lhsT=Cp[:, h, li * 128:(li + 1) * 128],
                                                  rhs=hb[:, h, :], start=False, stop=True),
                         reads=[Cp, hb], writes=[py])
                y_ = yo[li]
                s_ = sz[li]
                for h in range(2):
                    hs = slice(h * 64, (h + 1) * 64)
                    k.op("dve", lambda e: e.scalar_tensor_tensor(out=y_[:, hs], in0=xtok[:, lt + li, hs],
                                                                 scalar=par[:, 2, h:h + 1], in1=py[:, li, hs],
                                                                 op0=ALU.mult, op1=ALU.add),
                         reads=[xtok, par, py], writes=[y_])
                k.op("act", lambda e: e.activation(out=s_[:], in_=ztok[:, lt + li, :], func=AF.Silu),
                     reads=[ztok], writes=[s_])
                k.op("pool", lambda e: e.tensor_tensor(out=y_[:], in0=y_[:], in1=s_[:], op=ALU.mult),
                     reads=[y_, s_], writes=[y_])
                k.dma("sp", y_o[(t0 + li) * 128:(t0 + li + 1) * 128, :], y_[:], reads=[y_], writes=[y_o])
            for h in range(2):
                for i in range(2):
                    k.op("pe", lambda e: e.matmul(pst[:, h, :], lhsT=Btok[:, lt + i, :], rhs=xdp[:, h, i, :],
                                                  start=(i == 0), stop=(i == 1)), reads=[Btok, xdp], writes=[pst])
                k.op("dve", lambda e: e.scalar_tensor_tensor(out=hst[:, h, :], in0=hst[:, h, :], scalar=cd[:, h:h + 1],
                                                             in1=pst[:, h, :], op0=ALU.mult, op1=ALU.add),
                     reads=[hst, cd, pst], writes=[hst])
            k.op("act", lambda e: e.activation(out=hb[:], in_=hst[:], func=AF.Copy), reads=[hst], writes=[hb])
    k.finish([y_o], "sp")
    k.close()
    return nc


def ssd_consts():
    s = np.arange(256)[:, None]
    l = np.arange(256)[None, :]
    t = (s <= l).astype(np.float32).reshape(2, 128, 256).transpose(1, 0, 2)
    return np.ascontiguousarray(t), np.ascontiguousarray(np.where(t > 0, 0.0, NEG).astype(np.float32))


def build_C():
    nc = bass.Bass("TRN2", target_bir_lowering=False)
    k = KB(nc)
    EI, EO = "ExternalInput", "ExternalOutput"
    xT_d = k.dram("xT", [D, TOWN], F32, EI)
    hT_d = k.dram("hT", [D, TOWN], BF16, EI)
    ys_d = k.dram("yssm", [TOWN, 512], F32, EI)
    yb_d = [k.dram(n, [512, TOWN], BF16, EI) for n in ("yfT", "ymT", "ydT")]
    wg_d = k.dram("w_gate", [D, 4 * D], F32, EI)
    bg_d = k.dram("bgate", [128, 32], F32, EI)
    wb_d = k.dram("w_branch", [4, 512, D], F32, EI)
    wo_d = k.dram("w_out", [D, D], F32, EI)
    gn_d = k.dram("gains", [128, 3, 8], F32, EI)
    nw_d = k.dram("ssmnw", [128, 512], F32, EI)
    wfi_d = k.dram("w_ffn_in", [D, 2 * FFH], F32, EI)
    wfo_d = k.dram("w_ffn_out", [FFH, D], F32, EI)
    idb_d = k.dram("ident", [128, 128], BF16, EI)
    x2_o = k.dram("x2T", [D, TOWN], F32, EO)

    gn = k.sbuf("gn_s", [128, 3, 8], F32)
    bg = k.sbuf("bg_s", [128, 32], F32)
    ones_f = k.sbuf("ones_f", [128, 128], F32)
    identb = k.sbuf("identb", [128, 128], BF16)
    k.dma("sp", gn[:], gn_d[:], reads=[gn_d], writes=[gn])
    k.dma("sp", bg[:], bg_d[:], reads=[bg_d], writes=[bg])
    k.dma("sp", identb[:], idb_d[:], reads=[idb_d], writes=[identb])
    k.op("dve", lambda e: e.memset(ones_f[:], 1.0), writes=[ones_f])
    mergedT = k.sbuf("mergedT", [128, 8, TOWN], BF16)
    psA = [k.psum("psA%d" % i, [128, TT], F32) for i in range(2)]
    psB = [k.psum("psB%d" % i, [128, TT], F32) for i in range(2)]
    ps_r = k.psum("ps_r", [128, TT], F32)
    mk = k.mark()

    hT = k.sbuf("hT_s", [128, 8, TOWN], BF16)
    yT = k.sbuf("yT_s", [128, 16, TOWN], BF16)
    nw = k.sbuf("nw_s", [128, 512], F32)
    k.dma("sp", hT[:], chunked(hT_d[:]), reads=[hT_d], writes=[hT])
    k.dma("act", nw[:], nw_d[:], reads=[nw_d], writes=[nw])
    for n in range(3):
        k.dma("act", yT[:, (n + 1) * 4:(n + 2) * 4, :], chunked(yb_d[n][:]), reads=[yb_d[n]], writes=[yT])
    ysb = [k.sbuf("ysb%d" % i, [128, 512], F32) for i in range(2)]
    junk = k.sbuf("junk", [128, 256], F32)
    ss = k.sbuf("ss", [128, 2], F32)
    ynb = [k.sbuf("ynb%d" % i, [128, 512], BF16) for i in range(2)]
    ptb = k.psum("ptb", [128, 4, 128], BF16)
    for t in range(16):
        y_ = ysb[t % 2]
        n_ = ynb[t % 2]
        k.dma("sp", y_[:], ys_d[t * 128:(t + 1) * 128, :], reads=[ys_d], writes=[y_])
        for g in range(2):
            k.op("act", lambda e, g=g: e.activation(out=junk[:], in_=y_[:, g * 256:(g + 1) * 256], func=AF.Square,
                                                    accum_out=ss[:, g:g + 1]), reads=[y_], writes=[junk, ss])
        k.op("act", lambda e: e.activation(out=ss[:], in_=ss[:], func=AF.Sqrt, scale=1.0 / 256, bias=EPS),
             reads=[ss], writes=[ss])
        k.op("dve", lambda e: e.reciprocal(out=ss[:], in_=ss[:]), reads=[ss], writes=[ss])
        for g in range(2):
            gs = slice(g * 256, (g + 1) * 256)
            k.op("dve", lambda e, g=g, gs=gs: e.scalar_tensor_tensor(out=n_[:, gs], in0=y_[:, gs], scalar=ss[:, g:g + 1],
                                                                     in1=nw[:, gs], op0=ALU.mult, op1=ALU.mult),
                 reads=[y_, ss, nw], writes=[n_])
        for kc in range(4):
            k.op("pe", lambda e, kc=kc: e.transpose(out=ptb[:, kc, :], in_=n_[:, kc * 128:(kc + 1) * 128],
                                                    identity=identb[:]), reads=[n_, identb], writes=[ptb])
        k.op("act", lambda e, t=t: e.activation(out=yT[:, 0:4, t * 128:(t + 1) * 128], in_=ptb[:], func=AF.Copy),
             reads=[ptb], writes=[yT])
    wgt = [k.sbuf("wgt%d" % i, [128, 8, 4, 128], BF16) for i in range(2)]
    wbt = [k.sbuf("wbt%d" % i, [128, 4, 4, 128], BF16) for i in range(2)]
    gsb = [k.sbuf("gsb%d" % i, [128, TT], F32) for i in range(2)]
    macc = k.sbuf("macc", [128, TT], F32)
    prod = k.sbuf("prod", [128, TT], F32)
    ci = 0
    for dc in range(8):
        wg_ = wgt[dc % 2]
        wb_ = wbt[dc % 2]
        for n in range(4):
            k.dma("pool", wg_[:, :, n, :], chunked(wg_d[:, n * D + dc * 128:n * D + (dc + 1) * 128]),
                  reads=[wg_d], writes=[wg_])
            k.dma("pool", wb_[:, n, :, :], chunked(wb_d[n, :, dc * 128:(dc + 1) * 128]), reads=[wb_d], writes=[wb_])
        for tt in range(4):
            tok = slice(tt * TT, (tt + 1) * TT)
            for n in range(4):
                pg_ = psA[ci % 2]
                pp_ = psB[ci % 2]
                g_ = gsb[ci % 2]
                ci += 1
                for c in range(8):
                    k.op("pe", lambda e, c=c: e.matmul(pg_[:], lhsT=wg_[:, c, n, :], rhs=hT[:, c, tok],
                                                       start=(c == 0), stop=(c == 7)), reads=[wg_, hT], writes=[pg_])
                for c in range(4):
                    k.op("pe", lambda e, c=c: e.matmul(pp_[:], lhsT=wb_[:, n, c, :], rhs=yT[:, n * 4 + c, tok],
                                                       start=(c == 0), stop=(c == 3)), reads=[wb_, yT], writes=[pp_])
                k.op("act", lambda e: e.activation(out=g_[:], in_=pg_[:], func=AF.Sigmoid,
                                                   bias=bg[:, n * 8 + dc:n * 8 + dc + 1]), reads=[pg_, bg], writes=[g_])
                if n == 0:
                    k.op("dve", lambda e: e.tensor_tensor(out=macc[:], in0=g_[:], in1=pp_[:], op=ALU.mult),
                         reads=[g_, pp_], writes=[macc])
                else:
                    k.op("dve", lambda e: e.tensor_tensor(out=prod[:], in0=g_[:], in1=pp_[:], op=ALU.mult),
                         reads=[g_, pp_], writes=[prod])
                    if n < 3:
                        k.op("pool", lambda e: e.tensor_tensor(out=macc[:], in0=macc[:], in1=prod[:], op=ALU.add),
                             reads=[macc, prod], writes=[macc])
                    else:
                        k.op("pool", lambda e: e.tensor_tensor(out=mergedT[:, dc, tok], in0=macc[:], in1=prod[:],
                                                               op=ALU.add), reads=[macc, prod], writes=[mergedT])
    k.release(mk)

    xt = k.sbuf("xt", [128, 8, TT], F32)
    mixT = k.sbuf("mixT", [128, 8, TT], F32)
    x1T = k.sbuf("x1T", [128, 8, TT], F32)
    hhT = k.sbuf("hhT", [128, 8, TT], BF16)
    actT = k.sbuf("actT", [128, 22, TT], BF16)
    sq = k.sbuf("sq", [128, 8, TT], F32)
    rstd = k.sbuf("rstd", [128, TT], F32)
    tmpn = k.sbuf("tmpn", [128, TT], F32)
    sg = [k.sbuf("sg%d" % i, [128, TT], F32) for i in range(2)]
    wo = [k.sbuf("wo%d" % i, [128, 8, 128], BF16) for i in range(2)]
    wfi = [k.sbuf("wfi%d" % i, [128, 8, 2, 128], BF16) for i in range(2)]
    wfo = [k.sbuf("wfo%d" % i, [128, 22, 128], BF16) for i in range(2)]
    wi = 0

    def norm_res(src, gi, resid, dst):
        emit_rstd(k, src, TT, ones_f, sq, ps_r, rstd)
        for c in range(8):
            k.op("dve", lambda e, c=c: e.scalar_tensor_tensor(out=tmpn[:], in0=src[:, c, :], scalar=gn[:, gi, c:c + 1],
                                                              in1=rstd[:], op0=ALU.mult, op1=ALU.mult),
                 reads=[src, gn, rstd], writes=[tmpn])
            k.op("pool", lambda e, c=c: e.tensor_tensor(out=dst[:, c, :], in0=tmpn[:], in1=resid[:, c, :], op=ALU.add),
                 reads=[tmpn, resid], writes=[dst])

    for tt in range(4):
        tok = slice(tt * TT, (tt + 1) * TT)
        k.dma("sp", xt[:], chunked(xT_d[:, tok]), reads=[xT_d], writes=[xt])
        for oc in range(8):
            w_ = wo[wi % 2]
            p_ = psA[wi % 2]
            wi += 1
            k.dma("pool", w_[:], chunked(wo_d[:, oc * 128:(oc + 1) * 128]), reads=[wo_d], writes=[w_])
            for c in range(8):
                k.op("pe", lambda e, c=c: e.matmul(p_[:], lhsT=w_[:, c, :], rhs=mergedT[:, c, tok],
                                                   start=(c == 0), stop=(c == 7)), reads=[w_, mergedT], writes=[p_])
            k.op("act", lambda e, oc=oc: e.activation(out=mixT[:, oc, :], in_=p_[:], func=AF.Copy),
                 reads=[p_], writes=[mixT])
        norm_res(mixT, 0, xt, x1T)
        emit_rstd(k, x1T, TT, ones_f, sq, ps_r, rstd)
        for c in range(8):
            k.op("dve", lambda e, c=c: e.scalar_tensor_tensor(out=hhT[:, c, :], in0=x1T[:, c, :], scalar=gn[:, 1, c:c + 1],
                                                              in1=rstd[:], op0=ALU.mult, op1=ALU.mult),
                 reads=[x1T, gn, rstd], writes=[hhT])
        for hc in range(22):
            w_ = wfi[hc % 2]
            pg_ = psA[hc % 2]
            pu_ = psB[hc % 2]
            s_ = sg[hc % 2]
            k.dma("pool", w_[:, :, 0, :], chunked(wfi_d[:, hc * 128:(hc + 1) * 128]), reads=[wfi_d], writes=[w_])
            k.dma("pool", w_[:, :, 1, :], chunked(wfi_d[:, FFH + hc * 128:FFH + (hc + 1) * 128]), reads=[wfi_d],
                  writes=[w_])
            for c in range(8):
                k.op("pe", lambda e, c=c: e.matmul(pg_[:], lhsT=w_[:, c, 0, :], rhs=hhT[:, c, :], start=(c == 0),
                                                   stop=(c == 7)), reads=[w_, hhT], writes=[pg_])
            for c in range(8):
                k.op("pe", lambda e, c=c: e.matmul(pu_[:], lhsT=w_[:, c, 1, :], rhs=hhT[:, c, :], start=(c == 0),
                                                   stop=(c == 7)), reads=[w_, hhT], writes=[pu_])
            k.op("act", lambda e: e.activation(out=s_[:], in_=pg_[:], func=AF.Silu), reads=[pg_], writes=[s_])
            k.op("dve", lambda e, hc=hc: e.tensor_tensor(out=actT[:, hc, :], in0=s_[:], in1=pu_[:], op=ALU.mult),
                 reads=[s_, pu_], writes=[actT])
        for oc in range(8):
            w_ = wfo[oc % 2]
            p_ = psA[oc % 2]
            k.dma("pool", w_[:], chunked(wfo_d[:, oc * 128:(oc + 1) * 128]), reads=[wfo_d], writes=[w_])
            for c in range(22):
                k.op("pe", lambda e, c=c: e.matmul(p_[:], lhsT=w_[:, c, :], rhs=actT[:, c, :], start=(c == 0),
                                                   stop=(c == 21)), reads=[w_, actT], writes=[p_])
            k.op("act", lambda e, oc=oc: e.activation(out=mixT[:, oc, :], in_=p_[:], func=AF.Copy),
                 reads=[p_], writes=[mixT])
        norm_res(mixT, 2, x1T, xt)
        k.dma("sp", chunked(x2_o[:, tok]), xt[:], reads=[xt], writes=[x2_o])
    k.finish([x2_o], "sp")
    k.close()
    return nc


def _cc(a):
    return np.ascontiguousarray(a)


def kernel(**inp):
    x = np.asarray(inp["x"], np.float32)
    f = lambda n, l: np.asarray(inp[n][l], np.float32)
    POS = [own_positions(j) for j in range(4)]
    ROPE = [rope_tables_np(POS[j]) for j in range(4)]
    DIAG = [diag_tables(j) for j in range(4)]
    MOBA = [moba_tables(j) for j in range(4)]
    onehot = (np.arange(S)[None, :] // 256 == np.arange(32)[:, None]).astype(NPBF)
    identb = np.eye(128).astype(NPBF)
    identf = np.eye(128, dtype=np.float32)
    tri, negtri = ssd_consts()
    xT = [_cc(x[c // 4, POS[c % 4]].T) for c in range(8)]
    for l in range(DEPTH):
        w_in = _cc(f("w_in", l))
        w_sw = make_w_sw(w_in)
        gpre = _cc(f("norm_mix_pre", l).reshape(8, 128).T)
        fb = _cc(f("fox_fbias", l).reshape(8, 1))
        maps = []
        for c in range(8):
            cos2, sinS = ROPE[c % 4]
            maps.append({"xT": xT[c], "gpre": gpre, "w_in": w_in, "w_sw": w_sw, "cos2": cos2, "sinS": sinS,
                         "cos2s": _cc(cos2 * 0.125), "sinSs": _cc(sinS * 0.125), "fbias": fb})
        rA = run(build_A(), maps)
        rA = [{n: np.asarray(v) for n, v in r.items()} for r in rA]
        G = []
        for b in range(NB):
            g = {n: gather_fm(rA, n, b) for n in ("fkT", "mkT", "dkT", "ikT", "logfT", "zxbcT", "dtT")}
            for n in ("fv", "mv", "dv"):
                g[n] = gather_tm(rA, n, b)
            km = np.empty((512, 32), np.float32)
            for j in range(4):
                km[:, POS[j][::256] // 256] = rA[b * 4 + j]["kmT"]
            g["kmT_all"] = km
            G.append(g)
        yT = {}
        for kind, pre in (("fox", "f"), ("moba", "m"), ("dsa", "d")):
            maps = []
            for c in range(8):
                b, j = c // 4, c % 4
                mp = {"qT": rA[c][pre + "qT"], "kT": G[b][pre + "kT"], "v": G[b][pre + "v"]}
                if kind != "dsa":
                    mp["diagm"] = DIAG[j][0]
                if kind == "fox":
                    ind = np.zeros((8, 4, 16), np.float32)
                    for m in range(4):
                        ind[:, m, :4 * m + j] = 1.0
                    mp.update({"logf_all": G[b]["logfT"], "logf_own": rA[c]["logfT"], "ind": ind})
                if kind == "moba":
                    vn, v01, o01 = MOBA[j]
                    mp.update({"onehot": onehot, "kmT_all": G[b]["kmT_all"], "validneg": _cc(vn), "valid01": _cc(v01),
                               "own01": _cc(o01), "ident": identb})
                if kind == "dsa":
                    mp.update({"iqT": rA[c]["iqT"], "ikT_all": G[b]["ikT"], "iw": rA[c]["iw"], "diagneg": DIAG[j][1],
                               "identf": identf})
                maps.append(mp)
            r = run(build_attn(kind), maps)
            yT[kind] = [np.asarray(r[c]["yT"]) for c in range(8)]
        cw_all, cb_all = f("conv_w", l), f("conv_b", l)
        maps = []
        for c in range(8):
            b, hp = c // 4, c % 4
            g = hp // 2
            zx = G[b]["zxbcT"]
            chans = [np.arange(hp * 128, hp * 128 + 128), 512 + g * 128 + np.arange(128), 768 + g * 128 + np.arange(128)]
            convw = np.stack([cw_all[:, ch].T for ch in chans], 1)
            convb = np.stack([cb_all[ch] for ch in chans], 1)
            par = np.stack([f("dt_bias", l)[2 * hp:2 * hp + 2], f("a_log", l)[2 * hp:2 * hp + 2],
                            f("d_skip", l)[2 * hp:2 * hp + 2]], 0)
            maps.append({"xpre": _cc(zx[512 + hp * 128:512 + (hp + 1) * 128]),
                         "Bpre": _cc(zx[1024 + g * 128:1024 + (g + 1) * 128]),
                         "Cpre": _cc(zx[1280 + g * 128:1280 + (g + 1) * 128]),
                         "ztok": _cc(zx[hp * 128:(hp + 1) * 128].T),
                         "dtraw": _cc(G[b]["dtT"][2 * hp:2 * hp + 2].T),
                         "convw": _cc(convw.astype(np.float32)), "convb": _cc(convb.astype(np.float32)),
                         "par": _cc(np.broadcast_to(par[None], (128, 3, 2)).astype(np.float32)),
                         "tri": tri, "negtri": negtri, "identf": identf})
        rS = run(build_ssd(), maps)
        rS = [np.asarray(r["y"]) for r in rS]
        bg = _cc(f("b_gate", l).reshape(4, 8, 128).transpose(2, 0, 1).reshape(128, 32))
        gains = _cc(np.stack([f(n, l).reshape(8, 128).T for n in ("norm_mix_post", "norm_ffn_pre", "norm_ffn_post")], 1))
        nw = _cc(np.broadcast_to(f("ssm_norm", l)[None], (128, 512)))
        maps = []
        for c in range(8):
            b, j = c // 4, c % 4
            yssm = _cc(np.concatenate([rS[b * 4 + hp][POS[j]] for hp in range(4)], 1))
            maps.append({"xT": xT[c], "hT": rA[c]["hT"], "yssm": yssm, "yfT": yT["fox"][c], "ymT": yT["moba"][c],
                         "ydT": yT["dsa"][c], "w_gate": _cc(f("w_gate", l)), "bgate": bg,
                         "w_branch": _cc(f("w_branch", l)), "w_out": _cc(f("w_out", l)), "gains": gains, "ssmnw": nw,
                         "w_ffn_in": _cc(f("w_ffn_in", l)), "w_ffn_out": _cc(f("w_ffn_out", l)), "ident": identb})
        rC = run(build_C(), maps)
        xT = [_cc(np.asarray(r["x2T"])) for r in rC]
    out = np.empty((NB, S, D), np.float32)
    for c in range(8):
        out[c // 4, POS[c % 4]] = xT[c].T
    return out
```
